# Optimizing a Trainium2 kernel written in Bass

```python
import jax, jax.numpy as jnp
from jax import lax
import numpy as np

D_MODEL = 2048
BATCH = 1
SEQ = 16384
DEPTH = 2

N_MIXERS = 2
NORM_EPS = 1e-6
DN_HEAD_DIM = 128
DN_QK_HEADS = D_MODEL // DN_HEAD_DIM
DN_V_HEADS = 2 * DN_QK_HEADS
DN_QK_DIM = DN_QK_HEADS * DN_HEAD_DIM
DN_V_DIM = DN_V_HEADS * DN_HEAD_DIM
DN_CONV_CH = 2 * DN_QK_DIM + DN_V_DIM
DN_PROJ = DN_CONV_CH + DN_V_DIM + 2 * DN_V_HEADS
DN_CONV = 4
DN_CHUNK = 64
SGU_WIDTH = 2 * D_MODEL
SGU_CHUNK = 128
SGU_GROUPS = 32
SGU_GROUP_DIM = SGU_WIDTH // SGU_GROUPS
N_EXPERTS = 32
TOP_K = 4
EXPERT_DIM = D_MODEL
SWIGLU_LIMIT = 7.0
SWIGLU_ALPHA = 1.702
MOE_BLOCK = 128
N_DN_LAYERS = (DEPTH + 1) // 2
N_SGU_LAYERS = DEPTH // 2

kernel_name = "hybrid_gdn_sgu_moe_adaln"


def rms_norm(x, w):
    xf = x.astype(jnp.float32)
    y = xf * lax.rsqrt(jnp.mean(xf * xf, axis=-1, keepdims=True) + NORM_EPS)
    return (y * w).astype(x.dtype)


def layer_norm(x, w, b):
    xf = x.astype(jnp.float32)
    mu = jnp.mean(xf, axis=-1, keepdims=True)
    var = jnp.mean(jnp.square(xf - mu), axis=-1, keepdims=True)
    return ((xf - mu) * lax.rsqrt(var + NORM_EPS) * w + b).astype(x.dtype)


def l2_normalize(x):
    xf = x.astype(jnp.float32)
    return xf * lax.rsqrt(jnp.sum(xf * xf, axis=-1, keepdims=True) + NORM_EPS)


def causal_depthwise_conv(x, w):
    k, ch = w.shape
    return lax.conv_general_dilated(x, w[:, None, :].astype(x.dtype), window_strides=(1,),
                                    padding=[(k - 1, 0)], dimension_numbers=('NWC', 'WIO', 'NWC'),
                                    feature_group_count=ch)


def chunk_gated_delta_rule(q, k, v, g, beta):
    bsz, seq, nh, dk = q.shape
    dv = v.shape[-1]
    n_chunks = seq // DN_CHUNK

    def chunks(t):
        return t.reshape(bsz, n_chunks, DN_CHUNK, nh, -1).transpose(1, 0, 3, 2, 4)

    qc, kc, vc = chunks(q), chunks(k), chunks(v)
    gc = g.reshape(bsz, n_chunks, DN_CHUNK, nh).transpose(1, 0, 3, 2)
    bc = beta.reshape(bsz, n_chunks, DN_CHUNK, nh).transpose(1, 0, 3, 2)
    g_cum = jnp.cumsum(gc, axis=-1)
    causal = jnp.tril(jnp.ones((DN_CHUNK, DN_CHUNK), bool))
    strict = jnp.tril(jnp.ones((DN_CHUNK, DN_CHUNK), bool), k=-1)
    diff = g_cum[..., :, None] - g_cum[..., None, :]
    decay = jnp.where(causal, jnp.exp(jnp.where(causal, diff, 0.0)), 0.0)
    k_beta = kc * bc[..., None]
    v_beta = vc * bc[..., None]
    lower = jnp.where(strict, jnp.einsum('nbhid,nbhjd->nbhij', k_beta, kc) * decay, 0.0)
    a_mat = jnp.eye(DN_CHUNK, dtype=jnp.float32) + lower
    rhs = jnp.concatenate([v_beta, k_beta * jnp.exp(g_cum)[..., None]], axis=-1)
    sol = lax.linalg.triangular_solve(a_mat, rhs, left_side=True, lower=True, unit_diagonal=True)
    u, w = sol[..., :dv], sol[..., dv:]
    intra = jnp.where(causal, jnp.einsum('nbhid,nbhjd->nbhij', qc, kc) * decay, 0.0)

    def step(state, xs):
        q_i, k_i, u_i, w_i, g_i, a_i = xs
        v_new = u_i - jnp.einsum('bhck,bhkv->bhcv', w_i, state)
        o = (jnp.einsum('bhck,bhkv->bhcv', q_i * jnp.exp(g_i)[..., None], state)
             + jnp.einsum('bhij,bhjv->bhiv', a_i, v_new))
        g_last = g_i[..., -1]
        state = (state * jnp.exp(g_last)[..., None, None]
                 + jnp.einsum('bhck,bhcv->bhkv', k_i * jnp.exp(g_last[..., None] - g_i)[..., None], v_new))
        return state, o

    state0 = jnp.zeros((bsz, nh, dk, dv), jnp.float32)
    _, o = lax.scan(step, state0, (qc, kc, u, w, g_cum, intra))
    return o.transpose(1, 0, 3, 2, 4).reshape(bsz, seq, nh, dv)


def gated_deltanet(h, w_in, conv_w, a_log, dt_bias, o_norm_w, w_out):
    bsz, seq, _ = h.shape
    proj = h @ w_in
    qkv, z, a, b = jnp.split(proj, [DN_CONV_CH, DN_CONV_CH + DN_V_DIM,
                                    DN_CONV_CH + DN_V_DIM + DN_V_HEADS], axis=-1)
    qkv = jax.nn.silu(causal_depthwise_conv(qkv, conv_w))
    q, k, v = jnp.split(qkv, [DN_QK_DIM, 2 * DN_QK_DIM], axis=-1)
    rep = DN_V_HEADS // DN_QK_HEADS
    q = jnp.repeat(l2_normalize(q.reshape(bsz, seq, DN_QK_HEADS, DN_HEAD_DIM)), rep, axis=2) * (DN_HEAD_DIM ** -0.5)
    k = jnp.repeat(l2_normalize(k.reshape(bsz, seq, DN_QK_HEADS, DN_HEAD_DIM)), rep, axis=2)
    v = v.reshape(bsz, seq, DN_V_HEADS, DN_HEAD_DIM).astype(jnp.float32)
    beta = jax.nn.sigmoid(b.astype(jnp.float32))
    g = -jnp.exp(a_log.astype(jnp.float32)) * jax.nn.softplus(a.astype(jnp.float32) + dt_bias.astype(jnp.float32))
    o = chunk_gated_delta_rule(q, k, v, g, beta)
    o = rms_norm(o, o_norm_w) * jax.nn.silu(z.reshape(bsz, seq, DN_V_HEADS, DN_HEAD_DIM).astype(jnp.float32))
    return o.reshape(bsz, seq, DN_V_DIM).astype(h.dtype) @ w_out


def chunked_gmlp(h, w_in, b_in, ln_w, ln_b, w_sp, b_sp, w_out, b_out):
    bsz, seq, _ = h.shape
    n_chunks = seq // SGU_CHUNK
    zz = jax.nn.gelu(h @ w_in + b_in, approximate=False)
    u, v = jnp.split(zz, 2, axis=-1)
    v = layer_norm(v, ln_w, ln_b)
    vc = v.reshape(bsz, n_chunks, SGU_CHUNK, SGU_GROUPS, SGU_GROUP_DIM)
    w_causal = jnp.tril(w_sp)
    sp = jnp.einsum('gts,bnsgd->bntgd', w_causal, vc) + b_sp.T[None, None, :, :, None]
    return (u * sp.reshape(bsz, seq, SGU_WIDTH)) @ w_out + b_out


def moe_ffn(h, layer, w_router, b_router, w_gate_up, b_gate_up, w_down, b_down):
    bsz, seq, d = h.shape
    n_tok = bsz * seq
    xt = h.reshape(n_tok, d)
    logits = (xt @ w_router[layer] + b_router[layer]).astype(jnp.float32)
    top_val, top_idx = lax.top_k(logits, TOP_K)
    gates = jax.nn.softmax(top_val, axis=-1)
    flat_e = top_idx.reshape(-1)
    order = jnp.argsort(flat_e)
    sorted_e = flat_e[order]
    sorted_tok = order // TOP_K
    sorted_gate = gates.reshape(-1)[order]
    counts = jnp.bincount(flat_e, length=N_EXPERTS)
    padded = (counts + MOE_BLOCK - 1) // MOE_BLOCK * MOE_BLOCK
    start = jnp.cumsum(counts) - counts
    pend = jnp.cumsum(padded)
    pstart = pend - padded
    dest = pstart[sorted_e] + (jnp.arange(n_tok * TOP_K) - start[sorted_e])
    n_blocks = (n_tok * TOP_K + N_EXPERTS * (MOE_BLOCK - 1) + MOE_BLOCK - 1) // MOE_BLOCK
    n_rows = n_blocks * MOE_BLOCK
    row_tok = jnp.full((n_rows,), n_tok, jnp.int32).at[dest].set(sorted_tok.astype(jnp.int32))
    row_gate = jnp.zeros((n_rows,), jnp.float32).at[dest].set(sorted_gate)
    block_expert = jnp.clip(jnp.searchsorted(pend, jnp.arange(n_blocks) * MOE_BLOCK, side='right'),
                            0, N_EXPERTS - 1)
    x_pad = jnp.concatenate([xt, jnp.zeros((1, d), xt.dtype)], axis=0)
    xs = x_pad[row_tok].reshape(n_blocks, MOE_BLOCK, d)

    def expert_block(args):
        xb, e = args
        gu = xb @ w_gate_up[layer, e] + b_gate_up[layer, e]
        x_glu = jnp.minimum(gu[:, :EXPERT_DIM], SWIGLU_LIMIT)
        x_lin = jnp.clip(gu[:, EXPERT_DIM:], -SWIGLU_LIMIT, SWIGLU_LIMIT)
        act = x_glu * jax.nn.sigmoid(SWIGLU_ALPHA * x_glu) * (x_lin + 1.0)
        return act @ w_down[layer, e] + b_down[layer, e]

    ys = lax.map(expert_block, (xs, block_expert)).reshape(n_rows, d)
    out = jax.ops.segment_sum(ys.astype(jnp.float32) * row_gate[:, None], row_tok, num_segments=n_tok + 1)[:n_tok]
    return out.reshape(bsz, seq, d).astype(h.dtype)


def setup_inputs(seed: int = 0) -> dict:
    key = jax.random.key(seed)
    ks = jax.random.split(key, 32)

    def nrm(k, shape, scale):
        return scale * jax.random.normal(k, shape, jnp.float32)

    dt = jnp.exp(jax.random.uniform(ks[8], (N_DN_LAYERS, DN_V_HEADS), jnp.float32,
                                    minval=float(np.log(1e-3)), maxval=float(np.log(1e-1))))
    return {
        'x': nrm(ks[0], (BATCH, SEQ, D_MODEL), 1.0),
        'c': nrm(ks[1], (BATCH, D_MODEL), 1.0),
        'ada_w': nrm(ks[2], (DEPTH, D_MODEL, 6 * D_MODEL), 0.5 * D_MODEL ** -0.5),
        'ada_b': nrm(ks[3], (DEPTH, 6 * D_MODEL), 0.02),
        'norm_w': 1.0 + nrm(ks[4], (DEPTH, 2, D_MODEL), 0.05),
        'dn_w_in': nrm(ks[5], (N_DN_LAYERS, D_MODEL, DN_PROJ), D_MODEL ** -0.5),
        'dn_conv_w': nrm(ks[6], (N_DN_LAYERS, DN_CONV, DN_CONV_CH), DN_CONV ** -0.5),
        'dn_a_log': jnp.log(jax.random.uniform(ks[7], (N_DN_LAYERS, DN_V_HEADS), jnp.float32, minval=1.0, maxval=16.0)),
        'dn_dt_bias': dt + jnp.log(-jnp.expm1(-dt)),
        'dn_o_norm_w': 1.0 + nrm(ks[9], (N_DN_LAYERS, DN_HEAD_DIM), 0.05),
        'dn_w_out': nrm(ks[10], (N_DN_LAYERS, DN_V_DIM, D_MODEL), DN_V_DIM ** -0.5),
        'sgu_w_in': nrm(ks[11], (N_SGU_LAYERS, D_MODEL, 2 * SGU_WIDTH), D_MODEL ** -0.5),
        'sgu_b_in': nrm(ks[12], (N_SGU_LAYERS, 2 * SGU_WIDTH), 0.02),
        'sgu_ln_w': 1.0 + nrm(ks[13], (N_SGU_LAYERS, SGU_WIDTH), 0.05),
        'sgu_ln_b': nrm(ks[14], (N_SGU_LAYERS, SGU_WIDTH), 0.02),
        'sgu_w_sp': nrm(ks[15], (N_SGU_LAYERS, SGU_GROUPS, SGU_CHUNK, SGU_CHUNK), 0.5 * SGU_CHUNK ** -0.5),
        'sgu_b_sp': 1.0 + nrm(ks[16], (N_SGU_LAYERS, SGU_GROUPS, SGU_CHUNK), 0.1),
        'sgu_w_out': nrm(ks[17], (N_SGU_LAYERS, SGU_WIDTH, D_MODEL), SGU_WIDTH ** -0.5),
        'sgu_b_out': nrm(ks[18], (N_SGU_LAYERS, D_MODEL), 0.02),
        'moe_w_router': nrm(ks[19], (DEPTH, D_MODEL, N_EXPERTS), D_MODEL ** -0.5),
        'moe_b_router': nrm(ks[20], (DEPTH, N_EXPERTS), 0.01),
        'moe_w_gate_up': nrm(ks[21], (DEPTH, N_EXPERTS, D_MODEL, 2 * EXPERT_DIM), D_MODEL ** -0.5),
        'moe_b_gate_up': nrm(ks[22], (DEPTH, N_EXPERTS, 2 * EXPERT_DIM), 0.02),
        'moe_w_down': nrm(ks[23], (DEPTH, N_EXPERTS, EXPERT_DIM, D_MODEL), EXPERT_DIM ** -0.5),
        'moe_b_down': nrm(ks[24], (DEPTH, N_EXPERTS, D_MODEL), 0.02),
        'final_norm_w': 1.0 + nrm(ks[25], (D_MODEL,), 0.05),
    }


def reference(x, c, ada_w, ada_b, norm_w, dn_w_in, dn_conv_w, dn_a_log, dn_dt_bias, dn_o_norm_w, dn_w_out,
              sgu_w_in, sgu_b_in, sgu_ln_w, sgu_ln_b, sgu_w_sp, sgu_b_sp, sgu_w_out, sgu_b_out,
              moe_w_router, moe_b_router, moe_w_gate_up, moe_b_gate_up, moe_w_down, moe_b_down, final_norm_w):
    c_act = jax.nn.silu(c)
    for i in range(DEPTH):
        mod = (c_act @ ada_w[i] + ada_b[i])[:, None, :]
        sh1, sc1, gt1, sh2, sc2, gt2 = jnp.split(mod, 6, axis=-1)
        h = rms_norm(x, norm_w[i, 0]) * (1.0 + sc1) + sh1
        j = i // N_MIXERS
        if i % N_MIXERS == 0:
            y = gated_deltanet(h, dn_w_in[j], dn_conv_w[j], dn_a_log[j], dn_dt_bias[j], dn_o_norm_w[j], dn_w_out[j])
        else:
            y = chunked_gmlp(h, sgu_w_in[j], sgu_b_in[j], sgu_ln_w[j], sgu_ln_b[j], sgu_w_sp[j], sgu_b_sp[j],
                             sgu_w_out[j], sgu_b_out[j])
        x = x + gt1 * y
        h = rms_norm(x, norm_w[i, 1]) * (1.0 + sc2) + sh2
        x = x + gt2 * moe_ffn(h, i, moe_w_router, moe_b_router, moe_w_gate_up, moe_b_gate_up, moe_w_down, moe_b_down)
    return rms_norm(x, final_norm_w)
```

```python
import contextlib
import numpy as np
import concourse.bass as bass
import concourse.mybir as mybir
from concourse.bass_utils import run_bass_kernel_spmd

F32 = mybir.dt.float32
BF16 = mybir.dt.bfloat16
AF = mybir.ActivationFunctionType
ALU = mybir.AluOpType
AX = mybir.AxisListType
EPS = 1e-6


class Buf:
    __slots__ = ("name", "t", "w", "rs", "sem", "excl")

    def __init__(self, name, t=None):
        self.name = name
        self.t = t
        self.w = None
        self.rs = {}
        self.sem = None
        self.excl = False

    def __getitem__(self, idx):
        return self.t[idx]


class Stop(Exception):
    pass


class RR:
    def __init__(self, bufs):
        self.b = bufs
        self.i = 0

    def next(self):
        b = self.b[self.i % len(self.b)]
        self.i += 1
        return b


class K:
    def __init__(self, nc):
        self.nc = nc
        self.engs = {"pe": nc.tensor, "act": nc.scalar, "dve": nc.vector,
                     "pool": nc.gpsimd, "sp": nc.sync}
        self.sems = []
        self.semval = []
        self.esem = {}
        for e in self.engs:
            self.esem[e] = self._newsem("e_" + e)
        self.waited = {e: {} for e in self.engs}
        self.nops = 0
        self.stack = None
        self.dsems = {}
        self.names = {}
        self.stopped = False

    def _newsem(self, name):
        h = self.nc.alloc_semaphore(name=name)
        self.sems.append(h)
        self.semval.append(0)
        return len(self.sems) - 1

    def sb(self, name, shape, dt=F32):
        n = self.names.get(name, 0)
        self.names[name] = n + 1
        t = self.stack.enter_context(self.nc.sbuf_tensor("%s_%d" % (name, n), list(shape), dt))
        return Buf(name, t)

    def ps(self, name, shape, dt=F32):
        t = self.nc.alloc_psum_tensor(name, list(shape), dt)
        b = Buf(name, t)
        b.excl = True
        return b

    def dram(self, name, shape, dt=F32, kind="Internal"):
        t = self.nc.dram_tensor(name, list(shape), dt, kind=kind)
        return Buf(name, t)

    def _deps(self, reads, writes):
        toks = []
        for b in reads:
            if b.w is not None:
                toks.append(b.w)
            if b.excl:
                toks.extend(b.rs.items())
        for b in writes:
            if b.w is not None:
                toks.append(b.w)
            toks.extend(b.rs.items())
        return toks

    def _wait(self, ename, toks, skip_sem=None):
        eng = self.engs[ename]
        wd = self.waited[ename]
        need = {}
        for (s, v) in toks:
            if s == skip_sem:
                continue
            if wd.get(s, 0) >= v:
                continue
            if need.get(s, 0) < v:
                need[s] = v
        for s, v in need.items():
            eng.wait_ge(self.sems[s], v)
            wd[s] = v

    def _commit(self, tok, reads, writes):
        for b in reads:
            if b.rs.get(tok[0], 0) < tok[1]:
                b.rs[tok[0]] = tok[1]
        for b in writes:
            b.w = tok
            b.rs = {}

    def op(self, ename, fn, reads=(), writes=(), sig=True):
        if self.stopped:
            return None
        toks = self._deps(reads, writes)
        es = self.esem[ename]
        self._wait(ename, toks, skip_sem=es if ename == "pe" else None)
        ins = fn(self.engs[ename])
        self.nops += 1
        if sig:
            self.semval[es] += 1
            ins.then_inc(self.sems[es], 1)
            tok = (es, self.semval[es])
        else:
            tok = (es, self.semval[es] + 1)
        self._commit(tok, reads, writes)
        return tok

    def dma(self, qname, parts, reads=(), writes=(), key=None, **kw):
        if self.stopped:
            return None
        toks = self._deps(reads, writes)
        self._wait(qname, toks)
        kname = key if isinstance(key, str) else key.name
        if kname not in self.dsems:
            self.dsems[kname] = self._newsem("d_" + kname)
        s = self.dsems[kname]
        eng = self.engs[qname]
        for (o, i) in parts:
            eng.dma_start(out=o, in_=i, **kw).then_inc(self.sems[s], 16)
            self.semval[s] += 16
            self.nops += 1
        tok = (s, self.semval[s])
        self._commit(tok, reads, writes)
        return tok

    def collective(self, kind, op, in_buf, out_buf, ncores):
        if self.stopped:
            return None
        toks = self._deps([in_buf], [out_buf])
        self._wait("pool", toks)
        kname = "cc_" + out_buf.name
        if kname not in self.dsems:
            self.dsems[kname] = self._newsem("c_" + out_buf.name)
        s = self.dsems[kname]
        self.nc.gpsimd.collective_compute(
            kind, op, replica_groups=[list(range(ncores))],
            ins=[in_buf.t.ap().opt()], outs=[out_buf.t.ap().opt()],
        ).then_inc(self.sems[s], 1)
        self.semval[s] += 1
        tok = (s, self.semval[s])
        self._commit(tok, [in_buf], [out_buf])
        return tok

    def barrier(self):
        if self.stopped:
            return
        toks = [(s, v) for s, v in enumerate(self.semval) if v > 0]
        for e in self.engs:
            self._wait(e, toks, skip_sem=None)

    def mm(self, ps_ap, lhsT, rhs, start, stop, reads, writes, sig=True, lazy=False):
        if lazy:
            sig = stop
        return self.op("pe", lambda e: e.matmul(ps_ap, lhsT=lhsT, rhs=rhs, start=start, stop=stop),
                       reads, writes, sig)

    def tp(self, ps_ap, in_ap, ident_ap, reads, writes):
        return self.op("pe", lambda e: e.transpose(out=ps_ap, in_=in_ap, identity=ident_ap), reads, writes)

    def act(self, out, in_, func, reads, writes, bias=None, scale=None):
        kw = {}
        if bias is not None:
            kw["bias"] = bias
        if scale is not None:
            kw["scale"] = scale
        return self.op("act", lambda e: e.activation(out=out, in_=in_, func=func, **kw), reads, writes)

    def ts(self, eng, out, in0, s1, s2, op0, op1, reads, writes):
        if op1 is None:
            return self.op(eng, lambda e: e.tensor_scalar(out=out, in0=in0, scalar1=s1, scalar2=None, op0=op0),
                           reads, writes)
        return self.op(eng, lambda e: e.tensor_scalar(out=out, in0=in0, scalar1=s1, scalar2=s2, op0=op0, op1=op1),
                       reads, writes)

    def stt(self, out, in0, scalar, in1, op0, op1, reads, writes):
        return self.op("dve", lambda e: e.scalar_tensor_tensor(out=out, in0=in0, scalar=scalar, in1=in1,
                                                                op0=op0, op1=op1), reads, writes)

    def tt(self, eng, out, in0, in1, op, reads, writes):
        return self.op(eng, lambda e: e.tensor_tensor(out=out, in0=in0, in1=in1, op=op), reads, writes)

    def cp(self, eng, out, in_, reads, writes):
        if eng == "act":
            return self.act(out, in_, AF.Identity, reads, writes)
        return self.op(eng, lambda e: e.tensor_copy(out=out, in_=in_), reads, writes)


def make_cfg(D, S, NC):
    c = dict(D=D, S=S, NC=NC)
    c["KD"] = D // 128
    c["TL"] = S // NC
    c["E"] = 4 * NC
    c["F"] = D
    c["KF"] = D // 128
    c["W2"] = 2 * D
    c["G"] = (2 * D) // 128
    c["CPR"] = 6 * (D // 128) // NC
    c["TT"] = min(512, S // NC)
    c["GS"] = min(512, S // NC)
    assert D == 256 * NC
    return c


def build(cfg, debug=False):
    D, S, NC, KD, TL, E, F, KF, W2, G, CPR, TT, GS = (cfg[x] for x in
        ("D", "S", "NC", "KD", "TL", "E", "F", "KF", "W2", "G", "CPR", "TT", "GS"))
    nc = bass.Bass("TRN2", target_bir_lowering=False)
    k = K(nc)
    EI = "ExternalInput"
    x_in = k.dram("x_in", [TL, D], F32, EI)
    c_col = k.dram("c_col", [128, KD], F32, EI)
    ada_w = k.dram("ada_w", [2 * D, CPR * 128], F32, EI)
    ada_b = k.dram("ada_b", [128, 2 * CPR], F32, EI)
    normw = k.dram("normw", [128, 5 * KD], F32, EI)
    dn_wqkv = k.dram("dn_wqkv", [D, 1024], F32, EI)
    dn_wz = k.dram("dn_wz", [D, 512], F32, EI)
    dn_wab = k.dram("dn_wab", [D, 8], F32, EI)
    dn_cw = k.dram("dn_cw", [128, 32], F32, EI)
    dn_hp = k.dram("dn_hp", [1, 8], F32, EI)
    dn_onw = k.dram("dn_onw", [1, 128], F32, EI)
    dn_wout = k.dram("dn_wout", [512, D], F32, EI)
    sg_win = k.dram("sg_win", [D // NC, 2 * W2], F32, EI)
    sg_wout = k.dram("sg_wout", [W2 // NC, D], F32, EI)
    sg_wspT = k.dram("sg_wspT", [128, G * 128], F32, EI)
    sg_bin = k.dram("sg_bin", [1, 2 * W2], F32, EI)
    sg_lnw = k.dram("sg_lnw", [1, W2], F32, EI)
    sg_lnb = k.dram("sg_lnb", [1, W2], F32, EI)
    sg_bsp = k.dram("sg_bsp", [128, G], F32, EI)
    sg_bout = k.dram("sg_bout", [128, KD], F32, EI)
    mo_wr = k.dram("mo_wr", [2 * D, E], F32, EI)
    mo_br = k.dram("mo_br", [2, E], F32, EI)
    mo_wgu = k.dram("mo_wgu", [2 * 4 * D, 2 * F], F32, EI)
    mo_bgu = k.dram("mo_bgu", [128, 2 * 4 * 2 * KF], F32, EI)
    mo_wd = k.dram("mo_wd", [2 * 4 * F, D], F32, EI)
    mo_bd = k.dram("mo_bd", [2 * 4, D], F32, EI)
    sel4 = k.dram("sel4", [E, 4], F32, EI)
    selb = k.dram("selb", [E, 512], F32, EI)
    out = k.dram("out", [TL, D], F32, "ExternalOutput")
    dbg = {}
    if debug:
        for nm in ("dbg_x0", "dbg_x1", "dbg_x2", "dbg_x3"):
            dbg[nm] = k.dram(nm, [D, TL], F32, "ExternalOutput")
        dbg["dbg_y"] = k.dram("dbg_y", [D, TL], F32, "ExternalOutput")
        dbg["dbg_m"] = k.dram("dbg_m", [128, 1024], F32, "ExternalOutput")

    xT_d = k.dram("xT_d", [D, TL], F32)
    mod_in = k.dram("mod_in", [2 * CPR, 128], F32)
    mod_all = k.dram("mod_all", [NC * 2 * CPR, 128], F32)
    h_loc = k.dram("h_loc", [D, TL], BF16)
    h_all = k.dram("h_all", [NC * D, TL], BF16)
    g_loc = k.dram("g_loc", [E, TL], F32)
    g_all = k.dram("g_all", [NC * E, TL], F32)
    rs_in = k.dram("rs_in", [NC * D, TL], F32)
    rs_out = k.dram("rs_out", [D, TL], F32)
    y_sgu = k.dram("y_sgu", [D, TL], F32)
    wgu_t = k.dram("wgu_t", [2 * 4 * KF * 128, KD * 256], BF16)
    wd_t = k.dram("wd_t", [2 * 4 * KD * 128, KF * 128], BF16)
    swin_loc = k.dram("swin_loc", [D // NC, 2 * W2], BF16)
    swin_all = k.dram("swin_all", [D, 2 * W2], BF16)
    swout_loc = k.dram("swout_loc", [W2 // NC, D], BF16)
    swout_all = k.dram("swout_all", [W2, D], BF16)

    PS = [k.ps("ps%d" % i, [128, 512], F32) for i in range(8)]

    with contextlib.ExitStack() as gstack:
        k.stack = gstack
        ident = k.sb("ident", [128, 128], F32)
        identb = k.sb("identb", [128, 128], BF16)
        ones = k.sb("ones", [128, 128], F32)
        maskUI = k.sb("maskUI", [128, 128], F32)
        maskUS = k.sb("maskUS", [128, 128], F32)
        negmU = k.sb("negmU", [128, 128], F32)
        modc = k.sb("modc", [128, NC * 2 * CPR], F32)
        nwc = k.sb("nwc", [128, 5 * KD], F32)
        acol = k.sb("acol", [128, 4 * KD], F32)
        k.op("pool", lambda e: e.memset(ident[:, :], 0.0), writes=[ident])
        k.op("pool", lambda e: e.affine_select(out=ident[:, :], in_=ident[:, :], pattern=[[-1, 128]],
             compare_op=ALU.not_equal, fill=1.0, base=0, channel_multiplier=1), reads=[ident], writes=[ident])
        k.cp("dve", identb[:, :], ident[:, :], [ident], [identb])
        k.op("pool", lambda e: e.memset(ones[:, :], 1.0), writes=[ones])
        k.op("pool", lambda e: e.affine_select(out=maskUI[:, :], in_=ones[:, :], pattern=[[1, 128]],
             compare_op=ALU.is_ge, fill=0.0, base=0, channel_multiplier=-1), reads=[ones], writes=[maskUI])
        k.op("pool", lambda e: e.affine_select(out=maskUS[:, :], in_=ones[:, :], pattern=[[1, 128]],
             compare_op=ALU.is_gt, fill=0.0, base=0, channel_multiplier=-1), reads=[ones], writes=[maskUS])
        k.ts("dve", negmU[:, :], maskUI[:, :], -1.0, 30000.0, ALU.add, ALU.mult, [maskUI], [negmU])
        k.dma("sp", [(nwc[:, :], normw[:, :])], [normw], [nwc], key=nwc)

        def modcol(i, v, kk):
            gq = v * KD + kk
            r, q = gq // CPR, gq % CPR
            col = r * 2 * CPR + i * CPR + q
            return modc[:, col:col + 1]

        with contextlib.ExitStack() as st:
            k.stack = st
            cc = k.sb("cc", [128, KD], F32)
            adab = k.sb("adab", [128, 2 * CPR], F32)
            modp = k.sb("modp", [128, 2 * CPR], F32)
            awp = RR([k.sb("aw%d" % i, [128, KD, 128], F32) for i in range(2)])
            k.dma("sp", [(cc[:, :], c_col[:, :])], [c_col], [cc], key=cc)
            k.dma("sp", [(adab[:, :], ada_b[:, :])], [ada_b], [adab], key=adab)
            k.act(cc[:, :], cc[:, :], AF.Silu, [cc], [cc])
            for i in range(2):
                for q in range(CPR):
                    aw = awp.next()
                    k.dma("sp", [(aw[:, :, :], ada_w[i * D:(i + 1) * D, q * 128:(q + 1) * 128]
                                  .rearrange("(k p) n -> p k n", p=128))], [ada_w], [aw], key=aw)
                    ps = PS[(i * CPR + q) % 2]
                    for kk in range(KD):
                        k.mm(ps[:, 0:1], aw[:, kk, :], cc[:, kk:kk + 1], kk == 0, kk == KD - 1, [aw, cc], [ps])
                    k.tt("dve", modp[:, i * CPR + q:i * CPR + q + 1], ps[:, 0:1],
                         adab[:, i * CPR + q:i * CPR + q + 1], ALU.add, [ps, adab], [modp])
            mrow = k.sb("mrow", [2 * CPR, 128], F32)
            k.tp(PS[2][0:2 * CPR, 0:128], modp[:, :], ident[:, :], [modp, ident], [PS[2]])
            k.cp("dve", mrow[:, :], PS[2][0:2 * CPR, 0:128], [PS[2]], [mrow])
            k.dma("sp", [(mod_in[:, :], mrow[:, :])], [mrow], [mod_in], key=mrow)
            k.collective("AllGather", ALU.bypass, mod_in, mod_all, NC)
            RB = 4 * 2 * CPR
            nblk = (NC * 2 * CPR + RB - 1) // RB
            mall = RR([k.sb("mall%d" % i, [RB, 128], F32) for i in range(2)])
            for bi in range(nblk):
                r0 = bi * RB
                rr = min(RB, NC * 2 * CPR - r0)
                ma = mall.next()
                k.dma("sp", [(ma[0:rr, :], mod_all[r0:r0 + rr, :])], [mod_all], [ma], key=ma)
                k.tp(PS[3][:, 0:rr], ma[0:rr, :], ident[0:rr, 0:rr], [ma, ident], [PS[3]])
                k.cp("dve", modc[:, r0:r0 + rr], PS[3][:, 0:rr], [PS[3]], [modc])
            for i in range(2):
                for j in range(2):
                    for kk in range(KD):
                        c0 = (i * 2 + j) * KD + kk
                        k.stt(acol[:, c0:c0 + 1], modcol(i, 1 + 3 * j, kk), 1.0, nwc[:, c0:c0 + 1],
                              ALU.add, ALU.mult, [modc, nwc], [acol])
            k.barrier()

        with contextlib.ExitStack() as st:
            k.stack = st
            xtp = RR([k.sb("xt%d" % i, [128, D], F32) for i in range(2)])
            xop = RR([k.sb("xo%d" % i, [128, KD, 128], F32) for i in range(2)])
            for tb in range(TL // 128):
                xt = xtp.next()
                k.dma("sp", [(xt[:, :], x_in[tb * 128:(tb + 1) * 128, :])], [x_in], [xt], key=xt)
                xo = xop.next()
                for kk in range(KD):
                    ps = PS[kk % 4]
                    k.tp(ps[:, 0:128], xt[:, kk * 128:(kk + 1) * 128], ident[:, :], [xt, ident], [ps])
                    k.cp("act" if kk % 2 else "dve", xo[:, kk, :], ps[:, 0:128], [ps], [xo])
                k.dma("pool", [(xT_d[:, tb * 128:(tb + 1) * 128].rearrange("(k p) t -> p k t", p=128), xo[:, :, :])],
                      [xo], [xT_d], key=xo)
            if debug:
                k.dma("sp", [(dbg["dbg_x0"][:, :], xT_d[:, :])], [xT_d], [dbg["dbg_x0"]], key="dbgx")
            k.barrier()

        def norm_phase(tag, upd, nrm, router):
            final = nrm is None
            y_src, li_y, gt_v = upd if upd is not None else (None, None, None)
            li, which = nrm if nrm is not None else (None, None)
            with contextlib.ExitStack() as st:
                k.stack = st
                NW = min(512, TL)
                xg = k.sb(tag + "xg", [128, KD, NW], F32)
                yg = k.sb(tag + "yg", [128, KD, NW], F32) if y_src is not None else None
                hb = k.sb(tag + "hb", [128, KD, NW], BF16) if not final else None
                tq = RR([k.sb(tag + "tq%d" % i, [128, NW], F32) for i in range(3)])
                rstd = k.sb(tag + "rstd", [128, NW], F32)
                if router:
                    wr = k.sb(tag + "wr", [128, KD, E], F32)
                    brb = k.sb(tag + "brb", [128, E], F32)
                    k.dma("sp", [(wr[:, :, :], mo_wr[li * D:(li + 1) * D, :].rearrange("(k p) e -> p k e", p=128))],
                          [mo_wr], [wr], key=wr)
                    k.dma("sp", [(brb[:, :], mo_br[li:li + 1, :].broadcast_to([128, E]))], [mo_br], [brb], key=brb)
                    lg = RR([k.sb(tag + "lg%d" % i, [128, E], F32) for i in range(2)])
                    ex = RR([k.sb(tag + "ex%d" % i, [128, E], F32) for i in range(2)])
                    m8 = RR([k.sb(tag + "m8%d" % i, [128, 8], F32) for i in range(2)])
                    sm = RR([k.sb(tag + "sm%d" % i, [128, 4], F32) for i in range(2)])
                    gT = k.sb(tag + "gT", [E, NW], F32)
                if final:
                    ot = RR([k.sb(tag + "ot%d" % i, [128, D], F32) for i in range(2)])
                for gi in range(TL // NW):
                    cs = slice(gi * NW, (gi + 1) * NW)
                    k.dma("sp", [(xg[:, :, :], xT_d[:, cs].rearrange("(k p) t -> p k t", p=128))],
                          [xT_d], [xg], key=xg)
                    if y_src is not None:
                        k.dma("sp", [(yg[:, :, :], y_src[:, cs].rearrange("(k p) t -> p k t", p=128))],
                              [y_src], [yg], key=yg)
                        for kk in range(KD):
                            k.stt(xg[:, kk, :], yg[:, kk, :], modcol(li_y, gt_v, kk), xg[:, kk, :],
                                  ALU.mult, ALU.add, [yg, xg, modc], [xg])
                        k.dma("pool", [(xT_d[:, cs].rearrange("(k p) t -> p k t", p=128), xg[:, :, :])],
                              [xg], [xT_d], key=xg)
                    ssp = PS[0]
                    for kk in range(KD):
                        sq = tq.next()
                        k.act(sq[:, :], xg[:, kk, :], AF.Square, [xg], [sq])
                        k.mm(ssp[:, 0:NW], ones[:, :], sq[:, :], kk == 0, kk == KD - 1, [ones, sq], [ssp])
                    k.act(rstd[:, :], ssp[:, 0:NW], AF.Sqrt, [ssp], [rstd], bias=EPS, scale=1.0 / D)
                    k.op("dve", lambda e: e.reciprocal(out=rstd[:, :], in_=rstd[:, :]), [rstd], [rstd])
                    if final:
                        for kk in range(KD):
                            k.stt(xg[:, kk, :], xg[:, kk, :], nwc[:, 4 * KD + kk:4 * KD + kk + 1], rstd[:, :],
                                  ALU.mult, ALU.mult, [xg, nwc, rstd], [xg])
                        for tb in range(NW // 128):
                            o = ot.next()
                            for kk in range(KD):
                                ps = PS[1 + kk % 4]
                                k.tp(ps[:, 0:128], xg[:, kk, tb * 128:(tb + 1) * 128], ident[:, :], [xg, ident], [ps])
                                k.cp("act" if kk % 2 else "dve", o[:, kk * 128:(kk + 1) * 128], ps[:, 0:128], [ps], [o])
                            r0 = gi * NW + tb * 128
                            k.dma("pool", [(out[r0:r0 + 128, :], o[:, :])], [o], [out], key=o)
                        continue
                    a0 = (li * 2 + which) * KD
                    lps = [PS[1 + i] for i in range(NW // 128)]
                    for kk in range(KD):
                        hf = tq.next()
                        k.tt("dve", hf[:, :], xg[:, kk, :], rstd[:, :], ALU.mult, [xg, rstd], [hf])
                        k.act(hf[:, :], hf[:, :], AF.Identity, [hf, acol, modc], [hf],
                              bias=modcol(li, 3 * which, kk), scale=acol[:, a0 + kk:a0 + kk + 1])
                        k.cp("pool", hb[:, kk, :], hf[:, :], [hf], [hb])
                        if router:
                            for tb in range(NW // 128):
                                k.mm(lps[tb][:, 0:E], hf[:, tb * 128:(tb + 1) * 128], wr[:, kk, :],
                                     kk == 0, kk == KD - 1, [hf, wr], [lps[tb]])
                    k.dma("pool", [(h_loc[:, cs].rearrange("(k p) t -> p k t", p=128), hb[:, :, :])],
                          [hb], [h_loc], key=hb)
                    if router:
                        for tb in range(NW // 128):
                            l_, e_, m_, s_ = lg.next(), ex.next(), m8.next(), sm.next()
                            k.tt("dve", l_[:, :], lps[tb][:, 0:E], brb[:, :], ALU.add, [lps[tb], brb], [l_])
                            k.op("dve", lambda e: e.max(out=m_[:, :], in_=l_[:, :]), [l_], [m_])
                            k.ts("dve", s_[:, 0:1], m_[:, 0:1], -1.0, None, ALU.mult, None, [m_], [s_])
                            k.act(e_[:, :], l_[:, :], AF.Exp, [l_, s_], [e_], bias=s_[:, 0:1])
                            k.stt(e_[:, :], l_[:, :], m_[:, 3:4], e_[:, :], ALU.is_ge, ALU.mult, [l_, m_, e_], [e_])
                            k.op("dve", lambda e: e.tensor_reduce(out=s_[:, 1:2], in_=e_[:, :], axis=AX.X, op=ALU.add),
                                 [e_], [s_])
                            k.op("dve", lambda e: e.reciprocal(out=s_[:, 2:3], in_=s_[:, 1:2]), [s_], [s_])
                            k.ts("dve", e_[:, :], e_[:, :], s_[:, 2:3], None, ALU.mult, None, [e_, s_], [e_])
                            pt = PS[5 + tb % 2]
                            k.tp(pt[0:E, 0:128], e_[:, :], ident[:, :], [e_, ident], [pt])
                            k.cp("act", gT[:, tb * 128:(tb + 1) * 128], pt[0:E, 0:128], [pt], [gT])
                        k.dma("pool", [(g_loc[:, cs], gT[:, :])], [gT], [g_loc], key=gT)
                k.barrier()

        def moe_prepass():
            with contextlib.ExitStack() as st:
                k.stack = st
                sg_ = RR([k.sb("ppsg%d" % i, [128, KD, 256], F32) for i in range(2)])
                sl_ = RR([k.sb("ppsl%d" % i, [128, KD, 256], F32) for i in range(2)])
                wb_ = RR([k.sb("ppwb%d" % i, [128, 2, KD, 256], BF16) for i in range(2)])
                sd_ = RR([k.sb("ppsd%d" % i, [128, KF, 256], F32) for i in range(2)])
                wdb_ = RR([k.sb("ppwdb%d" % i, [128, 2, KF, 128], BF16) for i in range(2)])
                n = 0
                for le in range(8):
                    for f2 in range(KF // 2):
                        sg, sl, wb = sg_.next(), sl_.next(), wb_.next()
                        src = mo_wgu[le * D:(le + 1) * D, :].rearrange("(k p) f -> p k f", p=128)
                        k.dma("sp", [(sg[:, :, :], src[:, :, f2 * 256:(f2 + 1) * 256])], [mo_wgu], [sg], key=sg)
                        k.dma("act", [(sl[:, :, :], src[:, :, F + f2 * 256:F + (f2 + 1) * 256])], [mo_wgu], [sl], key=sl)
                        for j in range(2):
                            k.cp("pool" if n % 2 else "dve", wb[:, j, :, 0:128], sg[:, :, j * 128:(j + 1) * 128], [sg], [wb])
                            k.cp("act", wb[:, j, :, 128:256], sl[:, :, j * 128:(j + 1) * 128], [sl], [wb])
                            n += 1
                        r0 = (le * KF + f2 * 2) * 128
                        k.dma("pool", [(wgu_t[r0:r0 + 256, :].rearrange("(j p) (k c) -> p j k c", p=128, c=256),
                                        wb[:, :, :, :])], [wb], [wgu_t], key=wb)
                    for d2 in range(KD // 2):
                        sd, wdb = sd_.next(), wdb_.next()
                        src = mo_wd[le * F:(le + 1) * F, :].rearrange("(k p) d -> p k d", p=128)
                        k.dma("sp", [(sd[:, :, :], src[:, :, d2 * 256:(d2 + 1) * 256])], [mo_wd], [sd], key=sd)
                        for j in range(2):
                            k.cp("pool" if j else "dve", wdb[:, j, :, :], sd[:, :, j * 128:(j + 1) * 128], [sd], [wdb])
                        r0 = (le * KD + d2 * 2) * 128
                        k.dma("pool", [(wd_t[r0:r0 + 256, :].rearrange("(j p) (k c) -> p j k c", p=128, c=128),
                                        wdb[:, :, :, :])], [wdb], [wd_t], key=wdb)
                stg = k.sb("ppst", [128, 2048], F32)
                stb = k.sb("ppstb", [128, 2048], BF16)

                def cast_rows(src, dst, nrows, ncols):
                    for r0 in range(0, nrows, 128):
                        rr = min(128, nrows - r0)
                        for c0 in range(0, ncols, 2048):
                            cw = min(2048, ncols - c0)
                            k.dma("sp", [(stg[0:rr, 0:cw], src[r0:r0 + rr, c0:c0 + cw])], [src], [stg], key=stg)
                            k.cp("dve", stb[0:rr, 0:cw], stg[0:rr, 0:cw], [stg], [stb])
                            k.dma("pool", [(dst[r0:r0 + rr, c0:c0 + cw], stb[0:rr, 0:cw])], [stb], [dst], key=stb)
                cast_rows(sg_win, swin_loc, D // NC, 2 * W2)
                cast_rows(sg_wout, swout_loc, W2 // NC, D)
                k.collective("AllGather", ALU.bypass, swin_loc, swin_all, NC)
                k.collective("AllGather", ALU.bypass, swout_loc, swout_all, NC)
                k.barrier()

        def moe_phase(li):
            with contextlib.ExitStack() as st:
                k.stack = st
                hT = k.sb("mhT", [128, KD, TT], BF16)
                acc = k.sb("macc", [128, KD, TT], F32)
                actT = k.sb("mact", [128, KF, TT], BF16)
                wgp = RR([k.sb("mwg%d" % i, [128, KD, 256], BF16) for i in range(2)])
                wdp = RR([k.sb("mwd%d" % i, [128, KF, 128], BF16) for i in range(2)])
                gbp = RR([k.sb("mgb%d" % i, [128, TT], F32) for i in range(2)])
                gtr = k.sb("mgtr", [E, TT], F32)
                g4 = k.sb("mg4", [4, TT], F32)
                bd4 = k.sb("mbd4", [4, D], F32)
                s4 = k.sb("ms4", [E, 4], F32)
                sbb = k.sb("msb", [E, 512], F32)
                bgu = k.sb("mbgu", [128, 4 * 2 * KF], F32)
                tA = RR([k.sb("mtA%d" % i, [128, TT], F32) for i in range(2)])
                tB = RR([k.sb("mtB%d" % i, [128, TT], F32) for i in range(2)])
                tC = RR([k.sb("mtC%d" % i, [128, TT], F32) for i in range(2)])
                k.dma("sp", [(bd4[:, :], mo_bd[li * 4:(li + 1) * 4, :])], [mo_bd], [bd4], key=bd4)
                k.dma("sp", [(s4[:, :], sel4[:, :])], [sel4], [s4], key=s4)
                k.dma("sp", [(sbb[:, :], selb[:, :])], [selb], [sbb], key=sbb)
                k.dma("sp", [(bgu[:, :], mo_bgu[:, li * 8 * KF:(li + 1) * 8 * KF])], [mo_bgu], [bgu], key=bgu)
                pidx = 0
                qsel = 0
                for ti in range(S // TT):
                    t0 = ti * TT
                    r, off = t0 // TL, t0 % TL
                    cs = slice(off, off + TT)
                    k.dma("sp", [(hT[:, :, :], h_all[r * D:(r + 1) * D, cs].rearrange("(k p) t -> p k t", p=128))],
                          [h_all], [hT], key=hT)
                    k.dma("sp", [(gtr[:, :], g_all[r * E:(r + 1) * E, cs])], [g_all], [gtr], key=gtr)
                    ps4 = PS[6]
                    k.mm(ps4[0:4, 0:TT], s4[:, :], gtr[:, :], True, True, [s4, gtr], [ps4])
                    k.cp("act", g4[:, :], ps4[0:4, 0:TT], [ps4], [g4])
                    for dc in range(KD):
                        ps = PS[4 + dc % 2]
                        k.mm(ps[:, 0:TT], bd4[:, dc * 128:(dc + 1) * 128], g4[:, :], True, True, [bd4, g4], [ps])
                        k.cp("act", acc[:, dc, :], ps[:, 0:TT], [ps], [acc])
                    for el in range(4):
                        gb = gbp.next()
                        psb = PS[7]
                        k.mm(psb[:, 0:TT], sbb[:, el * 128:(el + 1) * 128], gtr[:, :], True, True, [sbb, gtr], [psb])
                        k.cp("act", gb[:, :], psb[:, 0:TT], [psb], [gb])
                        for fc in range(KF):
                            wg = wgp.next()
                            r0 = ((li * 4 + el) * KF + fc) * 128
                            k.dma("sp" if qsel % 2 == 0 else "act",
                                  [(wg[:, :, :], wgu_t[r0:r0 + 128, :].rearrange("p (k c) -> p k c", c=256))],
                                  [wgu_t], [wg], key=wg)
                            qsel += 1
                            pg, pl = PS[pidx % 4], PS[(pidx + 1) % 4]
                            pidx += 2
                            for kk in range(KD):
                                k.mm(pg[:, 0:TT], wg[:, kk, 0:128], hT[:, kk, :], kk == 0, kk == KD - 1, [wg, hT], [pg], lazy=True)
                            for kk in range(KD):
                                k.mm(pl[:, 0:TT], wg[:, kk, 128:256], hT[:, kk, :], kk == 0, kk == KD - 1, [wg, hT], [pl], lazy=True)
                            a_, b_, c_ = tA.next(), tB.next(), tC.next()
                            bcol = el * 2 * KF
                            k.ts("dve", a_[:, :], pg[:, 0:TT], bgu[:, bcol + fc:bcol + fc + 1], 7.0, ALU.add, ALU.min,
                                 [pg, bgu], [a_])
                            k.act(b_[:, :], a_[:, :], AF.Sigmoid, [a_], [b_], scale=1.702)
                            k.ts("dve", c_[:, :], pl[:, 0:TT], bgu[:, bcol + KF + fc:bcol + KF + fc + 1], 7.0,
                                 ALU.add, ALU.min, [pl, bgu], [c_])
                            k.ts("pool", c_[:, :], c_[:, :], -7.0, 1.0, ALU.max, ALU.add, [c_], [c_])
                            k.tt("pool", a_[:, :], a_[:, :], b_[:, :], ALU.mult, [a_, b_], [a_])
                            k.tt("pool", c_[:, :], c_[:, :], gb[:, :], ALU.mult, [c_, gb], [c_])
                            k.tt("dve", actT[:, fc, :], a_[:, :], c_[:, :], ALU.mult, [a_, c_], [actT])
                        for dc in range(KD):
                            wd = wdp.next()
                            r0 = ((li * 4 + el) * KD + dc) * 128
                            k.dma("sp" if qsel % 2 == 0 else "act",
                                  [(wd[:, :, :], wd_t[r0:r0 + 128, :].rearrange("p (k c) -> p k c", c=128))],
                                  [wd_t], [wd], key=wd)
                            qsel += 1
                            py = PS[4 + dc % 2]
                            for fc in range(KF):
                                k.mm(py[:, 0:TT], wd[:, fc, :], actT[:, fc, :], fc == 0, fc == KF - 1, [wd, actT], [py], lazy=True)
                            k.tt("dve", acc[:, dc, :], py[:, 0:TT], acc[:, dc, :], ALU.add, [py, acc], [acc])
                    k.dma("pool", [(rs_in[r * D:(r + 1) * D, cs].rearrange("(k p) t -> p k t", p=128), acc[:, :, :])],
                          [acc], [rs_in], key=acc)
                k.collective("ReduceScatter", ALU.add, rs_in, rs_out, NC)
                k.barrier()

        def dn_phase():
            with contextlib.ExitStack() as st:
                k.stack = st
                NCH = GS // 128
                Wqkv = k.sb("dWqkv", [128, KD, 1024], BF16)
                Wz = k.sb("dWz", [128, KD, 512], BF16)
                Wab = k.sb("dWab", [128, KD, 8], BF16)
                Wout = k.sb("dWout", [128, 4, D], BF16)
                k.dma("pool", [(Wqkv[:, kk, :], dn_wqkv[kk * 128:(kk + 1) * 128, :]) for kk in range(KD)],
                      [dn_wqkv], [Wqkv], key=Wqkv)
                k.dma("pool", [(Wz[:, kk, :], dn_wz[kk * 128:(kk + 1) * 128, :]) for kk in range(KD)],
                      [dn_wz], [Wz], key=Wz)
                k.dma("pool", [(Wab[:, kk, :], dn_wab[kk * 128:(kk + 1) * 128, :]) for kk in range(KD)],
                      [dn_wab], [Wab], key=Wab)
                k.dma("pool", [(Wout[:, hv, :], dn_wout[hv * 128:(hv + 1) * 128, :]) for hv in range(4)],
                      [dn_wout], [Wout], key=Wout)
                cw = k.sb("dcw", [128, 32], F32)
                hp = k.sb("dhp", [128, 8], F32)
                negA = k.sb("dnegA", [128, 4], F32)
                onw = k.sb("donw", [128, 128], F32)
                k.dma("sp", [(cw[:, :], dn_cw[:, :])], [dn_cw], [cw], key=cw)
                k.dma("sp", [(hp[:, :], dn_hp[0:1, :].broadcast_to([128, 8]))], [dn_hp], [hp], key=hp)
                k.dma("sp", [(onw[:, :], dn_onw[0:1, :].broadcast_to([128, 128]))], [dn_onw], [onw], key=onw)
                k.act(negA[:, :], hp[:, 0:4], AF.Exp, [hp], [negA])
                k.ts("dve", negA[:, :], negA[:, :], -1.0, None, ALU.mult, None, [negA], [negA])
                PRE = [k.sb("dpre%d" % j, [128, 3 + GS], F32) for j in range(8)]
                QKV = [k.sb("dqkv%d" % j, [128, GS], F32) for j in range(8)]
                Sst = [k.sb("dS%d" % h, [128, 128], F32) for h in range(4)]
                for j in range(8):
                    k.op("pool", lambda e: e.memset(PRE[j][:, 0:3], 0.0), writes=[PRE[j]])
                for h in range(4):
                    k.op("pool", lambda e: e.memset(Sst[h][:, :], 0.0), writes=[Sst[h]])
                hT = k.sb("dhT", [128, KD, GS], BF16)
                tq = RR([k.sb("dtq%d" % i, [128, GS], F32) for i in range(3)])
                szp = RR([k.sb("dsz%d" % i, [128, 512], F32) for i in range(2)])
                sm = RR([k.sb("dsm%d" % i, [128, 32], F32) for i in range(2)])
                og = RR([k.sb("dog%d" % i, [128, 512], F32) for i in range(2)])
                ogT = k.sb("dogT", [128, 4, GS], BF16)
                yst = RR([k.sb("dyst%d" % i, [128, GS], F32) for i in range(2)])
                ktk = [k.sb("dktk%d" % i, [128, 128], F32) for i in range(2)]

                def H(nm):
                    return [k.sb("d%s%d" % (nm, h), [128, 128], F32) for h in range(4)]
                Gbc, dtp, DT, EB, DTs, P0, IT, QgT, Kd, Vt, Q0, Z0, Z1 = (H(n) for n in (
                    "Gbc", "dtp", "DT", "EB", "DTs", "P0", "IT", "QgT", "Kd", "Vt", "Q0", "Z0", "Z1"))
                Pa, Pb, Qa, Qb, tmpv, vnw, osb = (H(n) for n in ("Pa", "Pb", "Qa", "Qb", "tmpv", "vnw", "osb"))
                colv = [k.sb("dcol%d" % h, [128, 8], F32) for h in range(4)]
                def Qv(b):
                    return [PS[b]] * 4
                B2, B3 = Qv(2), Qv(3)
                HB = [Qv(4 + h) for h in range(4)]

                def qs(q):
                    return slice(q * 128, (q + 1) * 128)
                inps = RR([PS[0], PS[1]])

                def ck(n):
                    if cfg.get("dnstop") == n:
                        k.stopped = True

                for gi in range(S // GS):
                    t0 = gi * GS
                    r, off = t0 // TL, t0 % TL
                    k.dma("sp", [(hT[:, :, :], h_all[r * D:(r + 1) * D, off:off + GS].rearrange("(k p) t -> p k t", p=128))],
                          [h_all], [hT], key=hT)
                    for j in range(8):
                        ps = inps.next()
                        for kk in range(KD):
                            k.mm(ps[:, 0:GS], Wqkv[:, kk, j * 128:(j + 1) * 128], hT[:, kk, :], kk == 0, kk == KD - 1,
                                 [Wqkv, hT], [ps], lazy=True)
                        pre = PRE[j]
                        k.cp("act", pre[:, 3:3 + GS], ps[:, 0:GS], [ps], [pre])
                        acc = tq.next()
                        k.ts("dve", acc[:, :], pre[:, 0:GS], cw[:, j * 4:j * 4 + 1], None, ALU.mult, None, [pre, cw], [acc])
                        for tap in range(1, 4):
                            k.stt(acc[:, :], pre[:, tap:tap + GS], cw[:, j * 4 + tap:j * 4 + tap + 1], acc[:, :],
                                  ALU.mult, ALU.add, [pre, cw, acc], [acc])
                        k.cp("pool", pre[:, 0:3], pre[:, GS:GS + 3], [pre], [pre])
                        k.act(QKV[j][:, :], acc[:, :], AF.Silu, [acc], [QKV[j]])
                    ck(1)
                    for j in range(4):
                        sq = tq.next()
                        k.tt("pool", sq[:, :], QKV[j][:, :], QKV[j][:, :], ALU.mult, [QKV[j]], [sq])
                        ps = inps.next()
                        k.mm(ps[:, 0:GS], ones[:, :], sq[:, :], True, True, [ones, sq], [ps])
                        rn = tq.next()
                        k.act(rn[:, :], ps[:, 0:GS], AF.Sqrt, [ps], [rn], bias=EPS)
                        k.op("dve", lambda e: e.reciprocal(out=rn[:, :], in_=rn[:, :]), [rn], [rn])
                        k.stt(QKV[j][:, :], QKV[j][:, :], (128.0 ** -0.5) if j < 2 else 1.0, rn[:, :],
                              ALU.mult, ALU.mult, [QKV[j], rn], [QKV[j]])
                    ck(2)
                    for ci in range(NCH):
                        cs = slice(ci * 128, (ci + 1) * 128)
                        zps = inps.next()
                        for kk in range(KD):
                            k.mm(zps[:, 0:512], hT[:, kk, cs], Wz[:, kk, :], kk == 0, kk == KD - 1, [hT, Wz], [zps], lazy=True)
                        sz = szp.next()
                        k.act(sz[:, :], zps[:, 0:512], AF.Silu, [zps], [sz])
                        abps = B2[0]
                        for kk in range(KD):
                            k.mm(abps[:, 0:8], hT[:, kk, cs], Wab[:, kk, :], kk == 0, kk == KD - 1, [hT, Wab], [abps], lazy=True)
                        s_ = sm.next()
                        k.tt("dve", s_[:, 0:4], abps[:, 0:4], hp[:, 4:8], ALU.add, [abps, hp], [s_])
                        k.act(s_[:, 0:4], s_[:, 0:4], AF.Exp, [s_], [s_])
                        k.act(s_[:, 0:4], s_[:, 0:4], AF.Ln, [s_], [s_], bias=1.0)
                        k.tt("dve", s_[:, 0:4], s_[:, 0:4], negA[:, :], ALU.mult, [s_, negA], [s_])
                        k.act(s_[:, 4:8], abps[:, 4:8], AF.Exp, [abps], [s_], scale=-1.0)
                        k.ts("dve", s_[:, 4:8], s_[:, 4:8], 1.0, None, ALU.add, None, [s_], [s_])
                        k.op("dve", lambda e: e.reciprocal(out=s_[:, 4:8], in_=s_[:, 4:8]), [s_], [s_])
                        k.ts("dve", s_[:, 8:12], s_[:, 4:8], -1.0, None, ALU.mult, None, [s_], [s_])
                        gcps = B2[1]
                        k.mm(gcps[:, 128:132], maskUI[:, :], s_[:, 0:4], True, True, [maskUI, s_], [gcps])
                        k.cp("act", s_[:, 12:16], gcps[:, 128:132], [gcps], [s_])
                        k.act(s_[:, 16:20], s_[:, 12:16], AF.Exp, [s_], [s_])
                        k.ts("dve", s_[:, 16:20], s_[:, 16:20], -1.0, None, ALU.mult, None, [s_], [s_])
                        ck(3)
                        for hq in range(2):
                            kT = QKV[2 + hq]
                            qT = QKV[hq]
                            k.mm(B3[2 * hq][:, qs(2 * hq)], kT[:, cs], kT[:, cs], True, True, [kT], [B3[2 * hq]])
                            k.mm(B3[2 * hq + 1][:, qs(2 * hq + 1)], kT[:, cs], qT[:, cs], True, True, [kT, qT], [B3[2 * hq + 1]])
                            k.tp(B2[2 + hq][:, qs(2 + hq)], kT[:, cs], ident[:, :], [kT, ident], [B2[2 + hq]])
                        ck(4)
                        for hv in range(4):
                            k.ts("dve", Gbc[hv][:, :], ones[:, :], s_[:, hv:hv + 1], None, ALU.mult, None, [ones, s_], [Gbc[hv]])
                        ck(41)
                        for hv in range(4):
                            A = HB[hv][0]
                            k.mm(A[:, qs(0)], Gbc[hv][:, :], maskUI[:, :], True, True, [Gbc[hv], maskUI], [A])
                            k.tp(HB[hv][1][:, qs(1)], QKV[4 + hv][:, cs], ident[:, :], [QKV[4 + hv], ident], [HB[hv][1]])
                        ck(42)
                        for hv in range(4):
                            A = HB[hv][0]
                            hq = hv // 2
                            if hv == 1:
                                ck(48)
                            if hv == 2:
                                ck(49)
                            k.stt(dtp[hv][:, :], A[:, qs(0)], s_[:, 12 + hv:13 + hv], negmU[:, :], ALU.subtract, ALU.add,
                                  [A, s_, negmU], [dtp[hv]])
                            k.act(DT[hv][:, :], dtp[hv][:, :], AF.Exp, [dtp[hv]], [DT[hv]])
                            k.act(EB[hv][:, :], A[:, qs(0)], AF.Exp, [A], [EB[hv]])
                            k.cp("act", colv[hv][:, 0:1], A[:, 127:128], [A], [colv[hv]])
                            k.act(colv[hv][:, 1:2], s_[:, 12 + hv:13 + hv], AF.Exp, [s_, colv[hv]], [colv[hv]],
                                  scale=-1.0, bias=colv[hv][:, 0:1])
                            if hv == cfg.get('dnhv', 0):
                                ck(43)
                            k.cp("act", Vt[hv][:, :], HB[hv][1][:, qs(1)], [HB[hv][1]], [Vt[hv]])
                            k.tt("pool", DTs[hv][:, :], DT[hv][:, :], maskUS[:, :], ALU.mult, [DT[hv], maskUS], [DTs[hv]])
                            k.stt(P0[hv][:, :], B3[2 * hq][:, qs(2 * hq)], s_[:, 8 + hv:9 + hv], DTs[hv][:, :],
                                  ALU.mult, ALU.mult, [B3[2 * hq], s_, DTs[hv]], [P0[hv]])
                            if hv == cfg.get('dnhv', 0):
                                ck(44)
                            k.tt("dve", IT[hv][:, :], B3[2 * hq + 1][:, qs(2 * hq + 1)], DT[hv][:, :], ALU.mult,
                                 [B3[2 * hq + 1], DT[hv]], [IT[hv]])
                            if hv == cfg.get('dnhv', 0):
                                ck(45)
                            k.tt("pool", QgT[hv][:, :], QKV[hq][:, cs], EB[hv][:, :], ALU.mult, [QKV[hq], EB[hv]], [QgT[hv]])
                            if hv == cfg.get('dnhv', 0):
                                ck(46)
                            k.ts("dve", Kd[hv][:, :], B2[2 + hq][:, qs(2 + hq)], colv[hv][:, 1:2], None, ALU.mult, None,
                                 [B2[2 + hq], colv[hv]], [Kd[hv]])
                            if hv == cfg.get('dnhv', 0):
                                ck(47)
                            k.tt("pool", Z0[hv][:, :], ident[:, :], P0[hv][:, :], ALU.add, [ident, P0[hv]], [Z0[hv]])
                        ck(5)
                        for hv in range(4):
                            k.tp(HB[hv][2][:, qs(2)], P0[hv][:, :], ident[:, :], [P0[hv], ident], [HB[hv][2]])
                        for hv in range(4):
                            k.cp("act", Q0[hv][:, :], HB[hv][2][:, qs(2)], [HB[hv][2]], [Q0[hv]])
                        Pp = [P0[h] for h in range(4)]
                        Qp = [Q0[h] for h in range(4)]
                        Zc = [Z0[h] for h in range(4)]
                        Zn = [Z1[h] for h in range(4)]
                        ck(51)
                        QQ = cfg.get("qq", 2)
                        for lvl in range(1, 7):
                            if lvl == 2:
                                ck(52)
                            ck(60 + lvl)
                            Qn = (Qa if lvl % 2 else Qb)
                            Pn = (Pa if lvl % 2 else Pb)
                            for hv in range(4):
                                k.mm(HB[hv][QQ][:, qs(QQ)], Pp[hv][:, :], Qp[hv][:, :], True, True, [Pp[hv], Qp[hv]], [HB[hv][QQ]])
                                if lvl < 6:
                                    k.mm(HB[hv][1][:, qs(1)], Qp[hv][:, :], Pp[hv][:, :], True, True, [Pp[hv], Qp[hv]], [HB[hv][1]])
                            ck(53)
                            if cfg.get("var") == 5:
                                k.barrier()
                            for hv in range(4):
                                if cfg.get("var", 0) != 2:
                                    k.cp("dve" if cfg.get("var", 0) == 4 else "act", Qn[hv][:, :], HB[hv][QQ][:, qs(QQ)], [HB[hv][QQ]], [Qn[hv]])
                                if lvl < 6 and cfg.get("var", 0) != 1:
                                    k.cp("dve", Pn[hv][:, :], HB[hv][1][:, qs(1)], [HB[hv][1]], [Pn[hv]])
                            if debug and lvl == 1 and gi == 0 and ci == 0:
                                for ii, tl_ in enumerate([P0[0], Q0[0], Qa[0], Z0[0], DT[0], EB[0], IT[0], Kd[0]]):
                                    k.dma("sp", [(dbg["dbg_m"][:, ii * 128:(ii + 1) * 128], tl_[:, :])], [tl_], [dbg["dbg_m"]], key="dbgm")
                            ck(54)
                            for hv in range(4):
                                k.mm(HB[hv][3][:, qs(3)], Qn[hv][:, :], Zc[hv][:, :], True, True, [Qn[hv], Zc[hv]], [HB[hv][3]])
                            ck(55)
                            for hv in range(4):
                                k.tt("dve", Zn[hv][:, :], HB[hv][3][:, qs(3)], Zc[hv][:, :], ALU.add, [HB[hv][3], Zc[hv]], [Zn[hv]])
                            Pp = [Pn[h] for h in range(4)]
                            Qp = [Qn[h] for h in range(4)]
                            Zc, Zn = Zn, Zc
                        ck(6)
                        for hv in range(4):
                            kT = QKV[2 + hv // 2]
                            k.mm(HB[hv][0][:, qs(0)], kT[:, cs], Sst[hv][:, :], True, True, [kT, Sst[hv]], [HB[hv][0]])
                        for hv in range(4):
                            k.stt(tmpv[hv][:, :], HB[hv][0][:, qs(0)], s_[:, 16 + hv:17 + hv], Vt[hv][:, :], ALU.mult, ALU.add,
                                  [HB[hv][0], s_, Vt[hv]], [tmpv[hv]])
                        for hv in range(4):
                            k.mm(HB[hv][1][:, qs(1)], Zc[hv][:, :], tmpv[hv][:, :], True, True, [Zc[hv], tmpv[hv]], [HB[hv][1]])
                        for hv in range(4):
                            k.ts("dve", vnw[hv][:, :], HB[hv][1][:, qs(1)], s_[:, 4 + hv:5 + hv], None, ALU.mult, None,
                                 [HB[hv][1], s_], [vnw[hv]])
                        for hv in range(4):
                            O_ = HB[hv][2]
                            k.mm(O_[:, qs(2)], QgT[hv][:, :], Sst[hv][:, :], True, False, [QgT[hv], Sst[hv]], [O_], sig=False)
                            k.mm(O_[:, qs(2)], IT[hv][:, :], vnw[hv][:, :], False, True, [IT[hv], vnw[hv]], [O_])
                            k.mm(HB[hv][3][:, qs(3)], Kd[hv][:, :], vnw[hv][:, :], True, True, [Kd[hv], vnw[hv]], [HB[hv][3]])
                        for hv in range(4):
                            k.stt(Sst[hv][:, :], Sst[hv][:, :], EB[hv][:, 127:128], HB[hv][3][:, qs(3)], ALU.mult, ALU.add,
                                  [Sst[hv], EB[hv], HB[hv][3]], [Sst[hv]])
                            k.cp("act", osb[hv][:, :], HB[hv][2][:, qs(2)], [HB[hv][2]], [osb[hv]])
                        ck(7)
                        og_ = og.next()
                        for hv in range(4):
                            k.tt("pool", tmpv[hv][:, :], osb[hv][:, :], osb[hv][:, :], ALU.mult, [osb[hv]], [tmpv[hv]])
                            k.op("dve", lambda e: e.tensor_reduce(out=colv[hv][:, 2:3], in_=tmpv[hv][:, :], axis=AX.X, op=ALU.add),
                                 [tmpv[hv]], [colv[hv]])
                            k.act(colv[hv][:, 3:4], colv[hv][:, 2:3], AF.Sqrt, [colv[hv]], [colv[hv]], bias=EPS, scale=1.0 / 128.0)
                            k.op("dve", lambda e: e.reciprocal(out=colv[hv][:, 3:4], in_=colv[hv][:, 3:4]), [colv[hv]], [colv[hv]])
                            k.stt(osb[hv][:, :], osb[hv][:, :], colv[hv][:, 3:4], onw[:, :], ALU.mult, ALU.mult,
                                  [osb[hv], colv[hv], onw], [osb[hv]])
                            k.tt("dve", og_[:, hv * 128:(hv + 1) * 128], osb[hv][:, :], sz[:, hv * 128:(hv + 1) * 128], ALU.mult,
                                 [osb[hv], sz], [og_])
                        for hv in range(4):
                            k.tp(HB[hv][0][:, qs(0)], og_[:, hv * 128:(hv + 1) * 128], ident[:, :], [og_, ident], [HB[hv][0]])
                        for hv in range(4):
                            k.cp("act", ogT[:, hv, cs], HB[hv][0][:, qs(0)], [HB[hv][0]], [ogT])
                    ck(8)
                    for dc in range(KD):
                        ps = inps.next()
                        for hv in range(4):
                            k.mm(ps[:, 0:GS], Wout[:, hv, dc * 128:(dc + 1) * 128], ogT[:, hv, :], hv == 0, hv == 3,
                                 [Wout, ogT], [ps], lazy=True)
                        ys = yst.next()
                        k.cp("act" if dc % 2 else "dve", ys[:, :], ps[:, 0:GS], [ps], [ys])
                        k.dma("pool", [(rs_in[r * D + dc * 128:r * D + (dc + 1) * 128, off:off + GS], ys[:, :])],
                              [ys], [rs_in], key=ys)
                ck(9)
                k.collective("ReduceScatter", ALU.add, rs_in, rs_out, NC)
                k.barrier()

        def sgu_phase():
            with contextlib.ExitStack() as st:
                k.stack = st
                WspT = k.sb("sWsp", [128, G, 128], BF16)
                big = k.sb("sbig", [128, W2], F32)
                lnw = k.sb("slnw", [128, W2], F32)
                lnb = k.sb("slnb", [128, W2], F32)
                bsp = k.sb("sbsp", [128, G], F32)
                bout = k.sb("sbout", [128, KD], F32)
                k.dma("sp", [(big[:, :], sg_wspT[:, :])], [sg_wspT], [big], key=big)
                for g in range(G):
                    k.tt("dve", WspT[:, g, :], big[:, g * 128:(g + 1) * 128], maskUI[:, :], ALU.mult, [big, maskUI], [WspT])
                k.dma("sp", [(lnw[:, :], sg_lnw[0:1, :].broadcast_to([128, W2]))], [sg_lnw], [lnw], key=lnw)
                k.dma("sp", [(lnb[:, :], sg_lnb[0:1, :].broadcast_to([128, W2]))], [sg_lnb], [lnb], key=lnb)
                k.dma("sp", [(bsp[:, :], sg_bsp[:, :])], [sg_bsp], [bsp], key=bsp)
                k.dma("sp", [(bout[:, :], sg_bout[:, :])], [sg_bout], [bout], key=bout)
                hTc = RR([k.sb("shT%d" % i, [128, KD, 128], BF16) for i in range(2)])
                win = RR([k.sb("swin%d" % i, [128, KD, 512], BF16) for i in range(2)])
                binb = RR([k.sb("sbin%d" % i, [128, 512], F32) for i in range(2)])
                t32 = RR([k.sb("st32%d" % i, [128, 512], F32) for i in range(2)])
                ut = RR([k.sb("sut%d" % i, [128, 512], F32) for i in range(2)])
                zv = k.sb("szv", [128, W2], F32)
                vn = k.sb("svn", [128, W2], BF16)
                prodT = k.sb("sprodT", [128, G, 128], BF16)
                wo = RR([k.sb("swo%d" % i, [128, G, 256], BF16) for i in range(2)])
                ysg = k.sb("sysg", [128, KD, 128], F32)
                stt_ = RR([k.sb("sst%d" % i, [128, 8], F32) for i in range(2)])
                psr = RR([PS[0], PS[1], PS[2]])
                psp_ = RR([PS[3], PS[4]])
                pst = RR([PS[5], PS[6]])
                q = 0

                def inproj(hT_, c0):
                    nonlocal q
                    w_ = win.next()
                    b_ = binb.next()
                    k.dma("sp" if q % 2 == 0 else "act",
                          [(w_[:, :, :], swin_all[:, c0:c0 + 512].rearrange("(k p) c -> p k c", p=128))],
                          [swin_all], [w_], key=w_)
                    q += 1
                    k.dma("act", [(b_[:, :], sg_bin[0:1, c0:c0 + 512].broadcast_to([128, 512]))], [sg_bin], [b_], key=b_)
                    ps = psr.next()
                    for kk in range(KD):
                        k.mm(ps[:, 0:512], hT_[:, kk, :], w_[:, kk, :], kk == 0, kk == KD - 1, [hT_, w_], [ps], lazy=True)
                    t_ = t32.next()
                    k.tt("dve", t_[:, :], ps[:, 0:512], b_[:, :], ALU.add, [ps, b_], [t_])
                    return t_

                for ci in range(TL // 128):
                    cs = slice(ci * 128, (ci + 1) * 128)
                    hT_ = hTc.next()
                    k.dma("sp", [(hT_[:, :, :], h_loc[:, cs].rearrange("(k p) t -> p k t", p=128))], [h_loc], [hT_], key=hT_)
                    for cg in range(W2 // 512):
                        t_ = inproj(hT_, W2 + cg * 512)
                        k.act(zv[:, cg * 512:(cg + 1) * 512], t_[:, :], AF.Gelu, [t_], [zv])
                    s_ = stt_.next()
                    k.op("dve", lambda e: e.tensor_reduce(out=s_[:, 0:1], in_=zv[:, :], axis=AX.X, op=ALU.add), [zv], [s_])
                    k.tt("pool", big[:, :], zv[:, :], zv[:, :], ALU.mult, [zv], [big])
                    k.op("dve", lambda e: e.tensor_reduce(out=s_[:, 1:2], in_=big[:, :], axis=AX.X, op=ALU.add), [big], [s_])
                    k.ts("dve", s_[:, 2:3], s_[:, 0:1], 1.0 / W2, None, ALU.mult, None, [s_], [s_])
                    k.ts("dve", s_[:, 3:4], s_[:, 1:2], 1.0 / W2, None, ALU.mult, None, [s_], [s_])
                    k.stt(s_[:, 4:5], s_[:, 2:3], s_[:, 2:3], s_[:, 3:4], ALU.mult, ALU.subtract, [s_], [s_])
                    k.ts("dve", s_[:, 5:6], s_[:, 4:5], -1.0, EPS, ALU.mult, ALU.add, [s_], [s_])
                    k.act(s_[:, 5:6], s_[:, 5:6], AF.Sqrt, [s_], [s_])
                    k.op("dve", lambda e: e.reciprocal(out=s_[:, 5:6], in_=s_[:, 5:6]), [s_], [s_])
                    k.stt(s_[:, 6:7], s_[:, 2:3], -1.0, s_[:, 5:6], ALU.mult, ALU.mult, [s_], [s_])
                    k.act(zv[:, :], zv[:, :], AF.Identity, [zv, s_], [zv], bias=s_[:, 6:7], scale=s_[:, 5:6])
                    k.tt("dve", zv[:, :], zv[:, :], lnw[:, :], ALU.mult, [zv, lnw], [zv])
                    k.tt("pool", vn[:, :], zv[:, :], lnb[:, :], ALU.add, [zv, lnb], [vn])
                    for cg in range(W2 // 512):
                        t_ = inproj(hT_, cg * 512)
                        u_ = ut.next()
                        k.act(u_[:, :], t_[:, :], AF.Gelu, [t_], [u_])
                        psp = psp_.next()
                        for j in range(4):
                            g = cg * 4 + j
                            k.mm(psp[:, j * 128:(j + 1) * 128], WspT[:, g, :], vn[:, g * 128:(g + 1) * 128], True, True,
                                 [WspT, vn], [psp])
                        for j in range(4):
                            g = cg * 4 + j
                            k.stt(big[:, g * 128:(g + 1) * 128], psp[:, j * 128:(j + 1) * 128], bsp[:, g:g + 1],
                                  u_[:, j * 128:(j + 1) * 128], ALU.add, ALU.mult, [psp, bsp, u_], [big])
                    for g4_ in range(G // 4):
                        pt = pst.next()
                        for j in range(4):
                            g = g4_ * 4 + j
                            k.tp(pt[:, j * 128:(j + 1) * 128], big[:, g * 128:(g + 1) * 128], ident[:, :], [big, ident], [pt])
                        k.cp("act" if g4_ % 2 else "dve", prodT[:, g4_ * 4:(g4_ + 1) * 4, :],
                             pt[:, 0:512].rearrange("p (j t) -> p j t", j=4), [pt], [prodT])
                    for d2 in range(KD // 2):
                        w_ = wo.next()
                        k.dma("sp" if d2 % 2 == 0 else "act",
                              [(w_[:, :, :], swout_all[:, d2 * 256:(d2 + 1) * 256].rearrange("(j p) c -> p j c", p=128))],
                              [swout_all], [w_], key=w_)
                        for h in range(2):
                            dc = d2 * 2 + h
                            ps = psr.next()
                            for g in range(G):
                                k.mm(ps[:, 0:128], w_[:, g, h * 128:(h + 1) * 128], prodT[:, g, :], g == 0, g == G - 1,
                                     [w_, prodT], [ps], lazy=True)
                            k.ts("dve", ysg[:, dc, :], ps[:, 0:128], bout[:, dc:dc + 1], None, ALU.add, None, [ps, bout], [ysg])
                    k.dma("pool", [(y_sgu[:, cs].rearrange("(k p) t -> p k t", p=128), ysg[:, :, :])], [ysg], [y_sgu], key=ysg)
                k.barrier()

        def dump(name, src):
            if debug:
                k.dma("sp", [(dbg[name][:, :], src[:, :])], [src], [dbg[name]], key="dbgx")
        steps = [
            ("pre", lambda: moe_prepass()),
            ("n1a", lambda: norm_phase("n1", None, (0, 0), False)),
            ("n1", lambda: k.collective("AllGather", ALU.bypass, h_loc, h_all, NC)),
            ("dn", lambda: (dn_phase(), dump("dbg_y", rs_out))),
            ("n2", lambda: (norm_phase("n2", (rs_out, 0, 2), (0, 1), True), dump("dbg_x1", xT_d),
                            k.collective("AllGather", ALU.bypass, h_loc, h_all, NC),
                            k.collective("AllGather", ALU.bypass, g_loc, g_all, NC))),
            ("moe0", lambda: moe_phase(0)),
            ("n3", lambda: (norm_phase("n3", (rs_out, 0, 5), (1, 0), False), dump("dbg_x2", xT_d))),
            ("sgu", lambda: sgu_phase()),
            ("n4", lambda: (norm_phase("n4", (y_sgu, 1, 2), (1, 1), True), dump("dbg_x3", xT_d),
                            k.collective("AllGather", ALU.bypass, h_loc, h_all, NC),
                            k.collective("AllGather", ALU.bypass, g_loc, g_all, NC))),
            ("moe1", lambda: moe_phase(1)),
            ("n5", lambda: norm_phase("n5", (rs_out, 1, 5), None, False)),
        ]
        for nm, fn in steps:
            fn()
            if k.stopped:
                k.stopped = False
                k.barrier()
                break
            if cfg.get("stop") == nm:
                break
        k.barrier()
        k.stack = None
    return nc


def shard_inputs(cfg, x, c, ada_w, ada_b, norm_w, dn_w_in, dn_conv_w, dn_a_log, dn_dt_bias, dn_o_norm_w, dn_w_out,
                 sgu_w_in, sgu_b_in, sgu_ln_w, sgu_ln_b, sgu_w_sp, sgu_b_sp, sgu_w_out, sgu_b_out,
                 moe_w_router, moe_b_router, moe_w_gate_up, moe_b_gate_up, moe_w_down, moe_b_down, final_norm_w):
    D, S, NC, KD, TL, E, F, KF, W2, G, CPR = (cfg[x_] for x_ in ("D", "S", "NC", "KD", "TL", "E", "F", "KF", "W2", "G", "CPR"))
    f = np.float32
    A = np.ascontiguousarray

    def cols(v):
        return A(np.asarray(v, f).reshape(-1, 128).T)
    maps = []
    QD = D
    HVT = 2 * (D // 128)
    for cix in range(NC):
        m = {}
        m["x_in"] = A(x[0, cix * TL:(cix + 1) * TL, :], dtype=f)
        m["c_col"] = cols(c[0])
        cw_ = CPR * 128
        m["ada_w"] = A(np.concatenate([ada_w[i][:, cix * cw_:(cix + 1) * cw_] for i in range(2)], 0), dtype=f)
        m["ada_b"] = A(np.concatenate([cols(ada_b[i][cix * cw_:(cix + 1) * cw_]) for i in range(2)], 1))
        m["normw"] = A(np.concatenate([cols(norm_w[i, j]) for i in range(2) for j in range(2)] + [cols(final_norm_w)], 1))
        w = dn_w_in[0]
        qc = [np.arange((2 * cix + h) * 128, (2 * cix + h + 1) * 128) for h in range(2)]
        kc = [QD + q_ for q_ in qc]
        vc = [2 * QD + np.arange((4 * cix + h) * 128, (4 * cix + h + 1) * 128) for h in range(4)]
        qkv_cols = np.concatenate(qc + kc + vc)
        m["dn_wqkv"] = A(w[:, qkv_cols], dtype=f)
        zc = np.concatenate([4 * QD + np.arange((4 * cix + h) * 128, (4 * cix + h + 1) * 128) for h in range(4)])
        m["dn_wz"] = A(w[:, zc], dtype=f)
        ac = 6 * QD + 4 * cix + np.arange(4)
        bc = 6 * QD + HVT + 4 * cix + np.arange(4)
        m["dn_wab"] = A(w[:, np.concatenate([ac, bc])], dtype=f)
        cwj = dn_conv_w[0][:, qkv_cols]
        m["dn_cw"] = A(cwj.reshape(4, 8, 128).transpose(2, 1, 0).reshape(128, 32), dtype=f)
        m["dn_hp"] = A(np.concatenate([dn_a_log[0][4 * cix:4 * cix + 4], dn_dt_bias[0][4 * cix:4 * cix + 4]])[None, :], dtype=f)
        m["dn_onw"] = A(dn_o_norm_w[0][None, :], dtype=f)
        m["dn_wout"] = A(dn_w_out[0][4 * cix * 128:(4 * cix + 4) * 128, :], dtype=f)
        rw = D // NC
        m["sg_win"] = A(sgu_w_in[0][cix * rw:(cix + 1) * rw, :], dtype=f)
        ro = W2 // NC
        m["sg_wout"] = A(sgu_w_out[0][cix * ro:(cix + 1) * ro, :], dtype=f)
        m["sg_wspT"] = A(sgu_w_sp[0].transpose(2, 0, 1).reshape(128, G * 128), dtype=f)
        m["sg_bin"] = A(sgu_b_in[0][None, :], dtype=f)
        m["sg_lnw"] = A(sgu_ln_w[0][None, :], dtype=f)
        m["sg_lnb"] = A(sgu_ln_b[0][None, :], dtype=f)
        m["sg_bsp"] = A(sgu_b_sp[0].T, dtype=f)
        m["sg_bout"] = cols(sgu_b_out[0])
        m["mo_wr"] = A(moe_w_router.reshape(2 * D, E), dtype=f)
        m["mo_br"] = A(moe_b_router, dtype=f)
        es = slice(4 * cix, 4 * cix + 4)
        m["mo_wgu"] = A(moe_w_gate_up[:, es].reshape(2 * 4 * D, 2 * F), dtype=f)
        m["mo_bgu"] = A(moe_b_gate_up[:, es].reshape(2, 4, 2 * KF, 128).transpose(3, 0, 1, 2).reshape(128, -1), dtype=f)
        m["mo_wd"] = A(moe_w_down[:, es].reshape(2 * 4 * F, D), dtype=f)
        m["mo_bd"] = A(moe_b_down[:, es].reshape(2 * 4, D), dtype=f)
        s4 = np.zeros((E, 4), f)
        sb_ = np.zeros((E, 512), f)
        for el in range(4):
            s4[4 * cix + el, el] = 1.0
            sb_[4 * cix + el, el * 128:(el + 1) * 128] = 1.0
        m["sel4"] = s4
        m["selb"] = sb_
        maps.append(m)
    return maps


_NC_CACHE = {}


def run(cfg, inputs, debug=False):
    key = (cfg["D"], cfg["S"], cfg["NC"], debug, cfg.get("stop"), cfg.get("dnstop"), cfg.get("dnhv"), cfg.get("var"), cfg.get("qq"))
    if key not in _NC_CACHE:
        _NC_CACHE[key] = build(cfg, debug)
    nc = _NC_CACHE[key]
    maps = shard_inputs(cfg, **inputs)
    res = run_bass_kernel_spmd(nc, maps, core_ids=list(range(cfg["NC"])))
    return res.results


def kernel(**inputs):
    cfg = make_cfg(2048, 16384, 8)
    inputs = {k_: np.asarray(v) for k_, v in inputs.items()}
    res = run(cfg, inputs)
    outp = np.concatenate([r["out"] for r in res], axis=0)
    return outp.reshape(1, cfg["S"], cfg["D"]).astype(np.float32)
```
